# Optimizing a Trainium2 kernel written in Bass

```python
import jax, jax.numpy as jnp
from jax import lax
import numpy as np

D_MODEL = 1024
BATCH = 2
SEQ = 16384
DEPTH = 4

N_MIXERS = 4
HEAD_DIM = 64
N_Q_HEADS = D_MODEL // HEAD_DIM
N_KV_HEADS = 4
GQA_GROUP = N_Q_HEADS // N_KV_HEADS
ATTN_Q_DIM = N_Q_HEADS * HEAD_DIM
ATTN_KV_DIM = N_KV_HEADS * HEAD_DIM
ATTN_IN_DIM = ATTN_Q_DIM + 2 * ATTN_KV_DIM
SWA_WINDOW = 128
SWA_BLOCK = 128
MOBA_BLOCK = 256
MOBA_TOP_K = 3
MOBA_Q_CHUNK = 64
GLA_HEADS = 4
GLA_KEY_DIM = D_MODEL // (2 * GLA_HEADS)
GLA_VAL_DIM = D_MODEL // GLA_HEADS
GLA_GATE_RANK = 16
GLA_GATE_TEMP = 16.0
GLA_IN_DIM = 2 * GLA_HEADS * GLA_KEY_DIM + 2 * GLA_HEADS * GLA_VAL_DIM + GLA_GATE_RANK
HGRN_EXPAND = 128
HGRN_HEADS = D_MODEL // HGRN_EXPAND
HGRN_KEY_DIM = HGRN_EXPAND
HGRN_VAL_DIM = D_MODEL // HGRN_HEADS
HGRN_IN_DIM = 4 * D_MODEL
LIN_CHUNK = 64
D_FF = ((8 * D_MODEL + 3 * 256 - 1) // (3 * 256)) * 256
RMS_EPS = 1e-6

kernel_name = 'hybrid_swa_moba_gla_hgrn2_trunk'


def rms_norm(x, w):
    xf = x.astype(jnp.float32)
    y = xf * lax.rsqrt(jnp.mean(xf * xf, axis=-1, keepdims=True) + RMS_EPS)
    return (y * w.astype(jnp.float32)).astype(x.dtype)


def head_rms_norm(o, w):
    return o * lax.rsqrt(jnp.mean(o * o, axis=-1, keepdims=True) + RMS_EPS) * w.astype(jnp.float32)


def alibi_slopes(n_heads):
    return jnp.exp2(-8.0 * jnp.arange(1, n_heads + 1, dtype=jnp.float32) / n_heads)


def split_qkv(h, w_in):
    B, T, _ = h.shape
    proj = h @ w_in
    q = proj[..., :ATTN_Q_DIM].reshape(B, T, N_KV_HEADS, GQA_GROUP, HEAD_DIM)
    k = proj[..., ATTN_Q_DIM:ATTN_Q_DIM + ATTN_KV_DIM].reshape(B, T, N_KV_HEADS, HEAD_DIM)
    v = proj[..., ATTN_Q_DIM + ATTN_KV_DIM:].reshape(B, T, N_KV_HEADS, HEAD_DIM)
    return q, k, v


def sliding_window_sink_attention(h, w_in, sinks, w_out):
    B, T, _ = h.shape
    L = SWA_BLOCK
    nb = T // L
    q, k, v = split_qkv(h, w_in)
    qb = q.reshape(B, nb, L, N_KV_HEADS, GQA_GROUP, HEAD_DIM)

    def with_prev(a):
        a = a.reshape(B, nb, L, N_KV_HEADS, HEAD_DIM)
        prev = jnp.concatenate([jnp.zeros_like(a[:, :1]), a[:, :-1]], axis=1)
        return jnp.concatenate([prev, a], axis=2)

    kw, vw = with_prev(k), with_prev(v)
    s = jnp.einsum('bnqhgd,bnkhd->bnhgqk', qb, kw, preferred_element_type=jnp.float32) * (HEAD_DIM ** -0.5)
    dist = (L + jnp.arange(L))[:, None] - jnp.arange(2 * L)[None, :]
    band = (dist >= 0) & (dist < SWA_WINDOW)
    has_prev = (jnp.arange(nb)[:, None, None] > 0) | (jnp.arange(2 * L)[None, None, :] >= L)
    mask = band[None] & has_prev
    slopes = alibi_slopes(N_Q_HEADS).reshape(N_KV_HEADS, GQA_GROUP)
    s = s - slopes[:, :, None, None] * dist.astype(jnp.float32)
    s = jnp.where(mask[None, :, None, None], s, -jnp.inf)
    sink = sinks.astype(jnp.float32).reshape(N_KV_HEADS, GQA_GROUP)[:, :, None, None]
    m = jnp.maximum(jnp.max(s, axis=-1, keepdims=True), sink)
    p = jnp.exp(s - m)
    denom = jnp.sum(p, axis=-1, keepdims=True) + jnp.exp(sink - m)
    o = jnp.einsum('bnhgqk,bnkhd->bnqhgd', p / denom, vw.astype(jnp.float32))
    return o.reshape(B, T, ATTN_Q_DIM).astype(h.dtype) @ w_out


def moba_attention(h, w_in, w_out):
    B, T, _ = h.shape
    Lb, Qc = MOBA_BLOCK, MOBA_Q_CHUNK
    q, k, v = split_qkv(h, w_in)
    Tp = ((T + Lb - 1) // Lb) * Lb
    pad = Tp - T
    q = jnp.pad(q.astype(jnp.float32), ((0, 0), (0, pad), (0, 0), (0, 0), (0, 0))) * (HEAD_DIM ** -0.5)
    k = jnp.pad(k.astype(jnp.float32), ((0, 0), (0, pad), (0, 0), (0, 0)))
    v = jnp.pad(v.astype(jnp.float32), ((0, 0), (0, pad), (0, 0), (0, 0)))
    nblk, nq = Tp // Lb, Tp // Qc
    n_sel = min(MOBA_TOP_K, nblk)
    kb = k.reshape(B, nblk, Lb, N_KV_HEADS, HEAD_DIM).transpose(0, 3, 1, 2, 4)
    vb = v.reshape(B, nblk, Lb, N_KV_HEADS, HEAD_DIM).transpose(0, 3, 1, 2, 4)
    k_mean = jnp.mean(kb, axis=3)
    gate = jnp.einsum('bthgd,bhnd->bhtn', q, k_mean)
    t_blk = jnp.arange(Tp) // Lb
    past = jnp.arange(nblk)[None, :] < t_blk[:, None]
    gate = jnp.where(past, gate, -jnp.inf)
    _, sel_idx = lax.top_k(gate, n_sel)
    sel_valid = jnp.arange(n_sel)[None, :] < t_blk[:, None]
    q_chunks = q.reshape(B, nq, Qc, N_KV_HEADS, GQA_GROUP, HEAD_DIM).transpose(1, 0, 2, 3, 4, 5)
    idx_chunks = sel_idx.reshape(B, N_KV_HEADS, nq, Qc, n_sel).transpose(2, 0, 1, 3, 4)
    valid_chunks = sel_valid.reshape(nq, Qc, n_sel)
    slopes = alibi_slopes(N_Q_HEADS).reshape(N_KV_HEADS, GQA_GROUP)
    gather_blocks = jax.vmap(jax.vmap(lambda blocks, ix: blocks[ix]))

    def attend(args):
        c, qc, idx, valid = args
        t_q = c * Qc + jnp.arange(Qc)
        own = (c * Qc) // Lb
        k_own = lax.dynamic_index_in_dim(kb, own, axis=2, keepdims=False)
        v_own = lax.dynamic_index_in_dim(vb, own, axis=2, keepdims=False)
        k_sel = gather_blocks(kb, idx)
        v_sel = gather_blocks(vb, idx)
        s_sel = jnp.einsum('bqhgd,bhqnkd->bhgqnk', qc, k_sel)
        dist_sel = (t_q[:, None, None] - (idx[..., None] * Lb + jnp.arange(Lb))).astype(jnp.float32)
        s_sel = s_sel - slopes[None, :, :, None, None, None] * dist_sel[:, :, None]
        s_sel = jnp.where(valid[:, :, None], s_sel, -jnp.inf)
        s_own = jnp.einsum('bqhgd,bhkd->bhgqk', qc, k_own)
        dist_own = t_q[:, None] - (own * Lb + jnp.arange(Lb))[None, :]
        s_own = s_own - slopes[:, :, None, None] * dist_own.astype(jnp.float32)
        s_own = jnp.where(dist_own >= 0, s_own, -jnp.inf)
        s_all = jnp.concatenate([s_sel.reshape(B, N_KV_HEADS, GQA_GROUP, Qc, n_sel * Lb), s_own], axis=-1)
        p = jax.nn.softmax(s_all, axis=-1)
        p_sel = p[..., :n_sel * Lb].reshape(B, N_KV_HEADS, GQA_GROUP, Qc, n_sel, Lb)
        p_own = p[..., n_sel * Lb:]
        return (jnp.einsum('bhgqnk,bhqnkd->bqhgd', p_sel, v_sel)
                + jnp.einsum('bhgqk,bhkd->bqhgd', p_own, v_own))

    o = lax.map(attend, (jnp.arange(nq), q_chunks, idx_chunks, valid_chunks))
    o = o.transpose(1, 0, 2, 3, 4, 5).reshape(B, Tp, ATTN_Q_DIM)[:, :T]
    return o.astype(h.dtype) @ w_out


def chunked_gated_linear_attention(q, k, v, log_g):
    B, T, H, K = q.shape
    V = v.shape[-1]
    L = LIN_CHUNK
    nc = T // L

    def to_chunks(a):
        return a.astype(jnp.float32).reshape(B, nc, L, H, a.shape[-1]).transpose(1, 0, 3, 2, 4)

    causal = jnp.tril(jnp.ones((L, L), dtype=bool))

    def step(S, inp):
        qc, kc, vc, gc = inp
        b = jnp.cumsum(gc, axis=2)
        b_last = b[:, :, -1:, :]
        q_dec = qc * jnp.exp(b)
        attn = jnp.einsum('bhtk,bhsk->bhts', q_dec, kc * jnp.exp(-b))
        attn = jnp.where(causal, attn, 0.0)
        o = jnp.einsum('bhts,bhsv->bhtv', attn, vc) + jnp.einsum('bhtk,bhkv->bhtv', q_dec, S)
        S = S * jnp.exp(b_last).swapaxes(-1, -2) + jnp.einsum('bhsk,bhsv->bhkv', kc * jnp.exp(b_last - b), vc)
        return S, o

    S0 = jnp.zeros((B, H, K, V), jnp.float32)
    _, o = lax.scan(step, S0, (to_chunks(q), to_chunks(k), to_chunks(v), to_chunks(log_g)))
    return o.transpose(1, 0, 3, 2, 4).reshape(B, T, H, V)


def gla_mixer(h, w_in, w_decay_up, b_decay, out_norm, w_out):
    B, T, _ = h.shape
    proj = h @ w_in
    dk, dv = GLA_HEADS * GLA_KEY_DIM, GLA_HEADS * GLA_VAL_DIM
    q = proj[..., :dk].reshape(B, T, GLA_HEADS, GLA_KEY_DIM).astype(jnp.float32) * (GLA_KEY_DIM ** -0.5)
    k = proj[..., dk:2 * dk].reshape(B, T, GLA_HEADS, GLA_KEY_DIM)
    v = proj[..., 2 * dk:2 * dk + dv].reshape(B, T, GLA_HEADS, GLA_VAL_DIM)
    g = proj[..., 2 * dk + dv:2 * dk + 2 * dv].reshape(B, T, GLA_HEADS, GLA_VAL_DIM)
    a = proj[..., 2 * dk + 2 * dv:]
    log_alpha = jax.nn.log_sigmoid((a @ w_decay_up + b_decay).astype(jnp.float32)) / GLA_GATE_TEMP
    log_alpha = log_alpha.reshape(B, T, GLA_HEADS, GLA_KEY_DIM)
    o = chunked_gated_linear_attention(q, k, v, log_alpha)
    o = head_rms_norm(o, out_norm) * jax.nn.silu(g.astype(jnp.float32))
    return o.reshape(B, T, dv).astype(h.dtype) @ w_out


def hgrn2_mixer(h, w_in, lower_bound, out_norm, w_out):
    B, T, _ = h.shape
    proj = h @ w_in
    d = D_MODEL
    q = jax.nn.silu(proj[..., :d].astype(jnp.float32)) * (HGRN_KEY_DIM ** -0.5)
    f = lower_bound + (1.0 - lower_bound) * jax.nn.sigmoid(proj[..., d:2 * d].astype(jnp.float32))
    i = proj[..., 2 * d:3 * d]
    g = proj[..., 3 * d:]
    shp_k = (B, T, HGRN_HEADS, HGRN_KEY_DIM)
    o = chunked_gated_linear_attention(q.reshape(shp_k), (1.0 - f).reshape(shp_k),
                                       i.reshape(B, T, HGRN_HEADS, HGRN_VAL_DIM), jnp.log(f).reshape(shp_k))
    o = head_rms_norm(o, out_norm) * jax.nn.silu(g.astype(jnp.float32)).reshape(B, T, HGRN_HEADS, HGRN_VAL_DIM)
    return o.reshape(B, T, HGRN_HEADS * HGRN_VAL_DIM).astype(h.dtype) @ w_out


def swiglu_ffn(h, w_gate_up, w_down):
    gu = h @ w_gate_up
    return (jax.nn.silu(gu[..., :D_FF]) * gu[..., D_FF:]) @ w_down


def setup_inputs(seed: int = 0) -> dict:
    key = jax.random.key(seed)
    ks = jax.random.split(key, 20)
    n_a, n_b, n_c, n_d = [(DEPTH - kind + N_MIXERS - 1) // N_MIXERS for kind in range(N_MIXERS)]

    def normal(k, shape):
        return jax.random.normal(k, shape, jnp.float32)

    def dense(k, shape, fan_in):
        return normal(k, shape) * (fan_in ** -0.5)

    def gain(k, shape):
        return 1.0 + 0.02 * normal(k, shape)

    return {
        'x': normal(ks[0], (BATCH, SEQ, D_MODEL)),
        'norm_mix': gain(ks[1], (DEPTH, D_MODEL)),
        'norm_ffn': gain(ks[2], (DEPTH, D_MODEL)),
        'swa_w_in': dense(ks[3], (n_a, D_MODEL, ATTN_IN_DIM), D_MODEL),
        'swa_sinks': 0.5 * normal(ks[4], (n_a, N_Q_HEADS)),
        'swa_w_out': dense(ks[5], (n_a, ATTN_Q_DIM, D_MODEL), ATTN_Q_DIM),
        'moba_w_in': dense(ks[6], (n_b, D_MODEL, ATTN_IN_DIM), D_MODEL),
        'moba_w_out': dense(ks[7], (n_b, ATTN_Q_DIM, D_MODEL), ATTN_Q_DIM),
        'gla_w_in': dense(ks[8], (n_c, D_MODEL, GLA_IN_DIM), D_MODEL),
        'gla_w_decay_up': dense(ks[9], (n_c, GLA_GATE_RANK, GLA_HEADS * GLA_KEY_DIM), GLA_GATE_RANK),
        'gla_b_decay': 0.1 * normal(ks[10], (n_c, GLA_HEADS * GLA_KEY_DIM)),
        'gla_out_norm': gain(ks[11], (n_c, GLA_VAL_DIM)),
        'gla_w_out': dense(ks[12], (n_c, GLA_HEADS * GLA_VAL_DIM, D_MODEL), GLA_HEADS * GLA_VAL_DIM),
        'hgrn_w_in': dense(ks[13], (n_d, D_MODEL, HGRN_IN_DIM), D_MODEL),
        'hgrn_lb_logits': 0.5 * normal(ks[14], (DEPTH, HGRN_HEADS * HGRN_KEY_DIM)),
        'hgrn_out_norm': gain(ks[15], (n_d, HGRN_VAL_DIM)),
        'hgrn_w_out': dense(ks[16], (n_d, HGRN_HEADS * HGRN_VAL_DIM, D_MODEL), HGRN_HEADS * HGRN_VAL_DIM),
        'ffn_w_gate_up': dense(ks[17], (DEPTH, D_MODEL, 2 * D_FF), D_MODEL),
        'ffn_w_down': dense(ks[18], (DEPTH, D_FF, D_MODEL), D_FF),
        'final_norm': gain(ks[19], (D_MODEL,)),
    }


def reference(x, norm_mix, norm_ffn, swa_w_in, swa_sinks, swa_w_out, moba_w_in, moba_w_out,
              gla_w_in, gla_w_decay_up, gla_b_decay, gla_out_norm, gla_w_out,
              hgrn_w_in, hgrn_lb_logits, hgrn_out_norm, hgrn_w_out,
              ffn_w_gate_up, ffn_w_down, final_norm):
    lb_p = jax.nn.softmax(hgrn_lb_logits.astype(jnp.float32), axis=0)
    lb_table = jnp.cumsum(lb_p, axis=0) - lb_p[0]
    for i in range(DEPTH):
        kind, j = i % N_MIXERS, i // N_MIXERS
        h = rms_norm(x, norm_mix[i])
        if kind == 0:
            mix = sliding_window_sink_attention(h, swa_w_in[j], swa_sinks[j], swa_w_out[j])
        elif kind == 1:
            mix = moba_attention(h, moba_w_in[j], moba_w_out[j])
        elif kind == 2:
            mix = gla_mixer(h, gla_w_in[j], gla_w_decay_up[j], gla_b_decay[j], gla_out_norm[j], gla_w_out[j])
        else:
            mix = hgrn2_mixer(h, hgrn_w_in[j], lb_table[i], hgrn_out_norm[j], hgrn_w_out[j])
        x = x + mix.astype(x.dtype)
        x = x + swiglu_ffn(rms_norm(x, norm_ffn[i]), ffn_w_gate_up[i], ffn_w_down[i]).astype(x.dtype)
    return rms_norm(x, final_norm)
```

```python
import numpy as np
from contextlib import ExitStack
import ml_dtypes
import concourse.bass as bass
import concourse.mybir as mybir
from concourse.bass_utils import run_bass_kernel_spmd

F32 = mybir.dt.float32
BF16 = mybir.dt.bfloat16
AF = mybir.ActivationFunctionType
ALU = mybir.AluOpType
AX = mybir.AxisListType
NPBF = ml_dtypes.bfloat16

EPOCH = 12000
NDSEM = 24
NEG = -30000.0

D = 1024
DFF = 2816
TC = 4096
NT = TC // 128
G = 512
NG = TC // G
HALO = 128
SEQ = 16384
NCORE = 8


class Tok:
    __slots__ = ("w", "r")

    def __init__(self):
        self.w = None
        self.r = {}


class BK:
    def __init__(self):
        self.nc = bass.Bass("TRN2", target_bir_lowering=False)
        self.es0 = ExitStack()
        self.stack = [self.es0]
        nc = self.nc
        self.eng = {"pe": nc.tensor, "act": nc.scalar, "dve": nc.vector, "pool": nc.gpsimd, "sp": nc.sync}
        self.cnt = {e: 0 for e in ("pe", "act", "dve", "pool")}
        self.sems = {}
        self.waited = {e: {} for e in self.eng}
        self.dma_n = 0
        self.dsem = [self.es0.enter_context(nc.semaphore(f"dsem{i}")) for i in range(NDSEM)]
        self.out_events = []
        self.uid = 0

    def barrier(self):
        deps = {}
        for e, c in self.cnt.items():
            if c > 0:
                deps[(e, (c - 1) // EPOCH)] = (c - 1) % EPOCH + 1
        for slot in range(min(NDSEM, self.dma_n)):
            uses = (self.dma_n - 1 - slot) // NDSEM + 1
            deps[("d", slot)] = uses * 16
        for e in self.eng:
            self._waits(e, deps)

    def push(self):
        self.barrier()
        es = ExitStack()
        self.stack.append(es)
        return es

    def pop(self):
        self.barrier()
        self.stack.pop().close()

    def name(self, p):
        self.uid += 1
        return f"{p}_{self.uid}"

    def sb(self, name, shape, dt):
        return self.stack[-1].enter_context(self.nc.sbuf_tensor(self.name(name), list(shape), dt))

    def ps(self, name, shape, dt):
        return self.stack[-1].enter_context(self.nc.psum_tensor(self.name(name), list(shape), dt))

    def _sem(self, key):
        s = self.sems.get(key)
        if s is None:
            s = self.es0.enter_context(self.nc.semaphore(f"s_{key[0]}_{key[1]}"))
            self.sems[key] = s
        return s

    def _semh(self, key):
        return self.dsem[key[1]] if key[0] == "d" else self._sem(key)

    @staticmethod
    def _deps(reads, writes):
        deps = {}
        for t in reads:
            if t.w is not None:
                k, v = t.w
                if deps.get(k, 0) < v:
                    deps[k] = v
        for t in writes:
            if t.w is not None:
                k, v = t.w
                if deps.get(k, 0) < v:
                    deps[k] = v
            for k, v in t.r.items():
                if deps.get(k, 0) < v:
                    deps[k] = v
        return deps

    def _waits(self, e, deps, skip_self=False):
        eng = self.eng[e]
        wd = self.waited[e]
        for k, v in deps.items():
            if skip_self and k[0] == e:
                continue
            if wd.get(k, 0) >= v:
                continue
            eng.wait_ge(self._semh(k), v)
            wd[k] = v

    def op(self, e, fn, reads=(), writes=()):
        deps = self._deps(reads, writes)
        self._waits(e, deps, skip_self=(e == "pe"))
        c = self.cnt[e]
        key = (e, c // EPOCH)
        val = c % EPOCH + 1
        fn().then_inc(self._sem(key), 1)
        self.cnt[e] = c + 1
        ev = (key, val)
        for t in writes:
            t.w = ev
            t.r = {}
        for t in reads:
            if t.r.get(key, 0) < val:
                t.r[key] = val
        return ev

    def dma(self, q, out, in_, reads=(), writes=(), is_output=False, **kw):
        slot = self.dma_n % NDSEM
        use = self.dma_n // NDSEM
        self.dma_n += 1
        deps = self._deps(reads, writes)
        key = ("d", slot)
        if use > 0 and deps.get(key, 0) < use * 16:
            deps[key] = use * 16
        self._waits(q, deps)
        self.eng[q].dma_start(out=out, in_=in_, **kw).then_inc(self.dsem[slot], 16)
        ev = (key, (use + 1) * 16)
        for t in writes:
            t.w = ev
            t.r = {}
        for t in reads:
            if t.r.get(key, 0) < ev[1]:
                t.r[key] = ev[1]
        if is_output:
            self.out_events.append(ev)
        return ev

    def finish(self):
        for slot in range(min(NDSEM, self.dma_n)):
            uses = (self.dma_n - 1 - slot) // NDSEM + 1
            k = ("d", slot)
            if self.waited["sp"].get(k, 0) < uses * 16:
                self.eng["sp"].wait_ge(self.dsem[slot], uses * 16)
                self.waited["sp"][k] = uses * 16
        while self.stack:
            self.stack.pop().close()


class Ring:
    def __init__(self, bk, name, shape, dt, n, psum=False):
        mk = bk.ps if psum else bk.sb
        self.bufs = [mk(f"{name}{i}", shape, dt) for i in range(n)]
        self.toks = [Tok() for _ in range(n)]
        self.i = 0
        self.n = n

    def next(self):
        j = self.i % self.n
        self.i += 1
        return self.bufs[j], self.toks[j]


class Prog:
    def __init__(self, ext_in=(), ext_out=()):
        self.b = BK()
        self.nc = self.b.nc
        self.ext_in = set(ext_in)
        self.ext_out = set(ext_out)
        self.dram = {}
        self.dtoks = {}
        self.consts_loaded = False

    def dr(self, name, shape=None, dt=None):
        if name not in self.dram:
            kind = "ExternalInput" if name in self.ext_in else ("ExternalOutput" if name in self.ext_out else "Internal")
            self.dram[name] = self.nc.dram_tensor(name, list(shape), dt, kind=kind).ap()
        return self.dram[name]

    def dt_(self, name, idx=0):
        k = (name, idx)
        if k not in self.dtoks:
            self.dtoks[k] = Tok()
        return self.dtoks[k]

    def load_consts(self):
        b, nc = self.b, self.nc
        self.ident = b.sb("ident", [128, 128], BF16)
        self.t_ident = Tok()
        b.dma("sp", self.ident[:], self.dr("c_ident", [128, 128], BF16)[:, :], writes=[self.t_ident])
        self.ones = b.sb("ones", [128, 128], BF16)
        self.t_ones = Tok()
        b.op("dve", lambda: nc.vector.memset(self.ones[:], 1.0), writes=[self.t_ones])
        self.gains = b.sb("gains", [128, 9, D], F32)
        self.t_gains = Tok()
        nm = self.dr("norm_mix", [4, D], F32)
        nf = self.dr("norm_ffn", [4, D], F32)
        fn = self.dr("final_norm", [D], F32)
        for i in range(4):
            b.dma("sp", self.gains[:, i, :], nm[i:i + 1, :].partition_broadcast(128), writes=[self.t_gains])
            b.dma("sp", self.gains[:, 4 + i, :], nf[i:i + 1, :].partition_broadcast(128), writes=[self.t_gains])
        b.dma("sp", self.gains[:, 8, :], fn.rearrange("(o d) -> o d", o=1).partition_broadcast(128), writes=[self.t_gains])

    def cast_w(self, src_name, src_shape, dst_name, sel=None):
        b = self.b
        src = self.dr(src_name, src_shape, F32)
        K, N = src_shape[-2], src_shape[-1]
        dst = self.dr(dst_name, [K, N], BF16)
        s2 = src[sel] if sel is not None else src
        step = 256
        for k0 in range(0, K, step):
            k1 = min(K, k0 + step)
            b.dma("pool", dst[k0:k1, :], s2[k0:k1, :], writes=[self.dt_(dst_name, k0 // step)])
        return [self.dt_(dst_name, i) for i in range((K + step - 1) // step)]

    def norm_tile(self, xs_ap, t_x, gain_idx, hT, t_hT, col0, R):
        b, nc = self.b, self.nc
        junk, t_j = R["junk"].next()
        ss, t_ss = R["ss"].next()
        b.op("act", lambda: nc.scalar.activation(out=junk[:], in_=xs_ap, func=AF.Square, accum_out=ss[:, 0:1]),
             reads=[t_x], writes=[t_j, t_ss])
        b.op("act", lambda: nc.scalar.activation(out=ss[:, 1:2], in_=ss[:, 0:1], func=AF.Sqrt, scale=1.0 / D, bias=1e-6),
             reads=[t_ss], writes=[t_ss])
        b.op("dve", lambda: nc.vector.reciprocal(out=ss[:, 2:3], in_=ss[:, 1:2]), reads=[t_ss], writes=[t_ss])
        xn, t_xn = R["xn"].next()
        b.op("dve", lambda: nc.vector.scalar_tensor_tensor(out=xn[:], in0=xs_ap, scalar=ss[:, 2:3],
                                                           in1=self.gains[:, gain_idx, :], op0=ALU.mult, op1=ALU.mult),
             reads=[t_x, t_ss, self.t_gains], writes=[t_xn])
        pt, t_pt = R["ptr"].next()
        for c in range(8):
            b.op("pe", lambda c=c: nc.tensor.transpose(pt[:, c * 128:(c + 1) * 128], xn[:, c * 128:(c + 1) * 128], self.ident[:]),
                 reads=[t_xn, self.t_ident], writes=[t_pt])
        b.op("act", lambda: nc.scalar.copy(out=hT[:, :, col0:col0 + 128], in_=pt[:].rearrange("p (c t) -> p c t", c=8)),
             reads=[t_pt], writes=[t_hT])

    def norm_rings(self):
        b = self.b
        return {"junk": Ring(b, "junk", [128, D], BF16, 1), "ss": Ring(b, "ss", [128, 4], F32, 2),
                "xn": Ring(b, "xn", [128, D], BF16, 2), "ptr": Ring(b, "ptr", [128, D], BF16, 2, psum=True)}

    def stage_post(self, l, x_in, x_out, ot_name, wout_name, final=False, out_name=None, ffn=True, x_out2=None):
        b, nc = self.b, self.nc
        b.push()
        xin = self.dr(x_in, [TC, D], F32)
        xout = self.dr(x_out, [TC, D], F32)
        ot_d = self.dr(ot_name, [8, 128, TC], BF16)
        wout_d = self.dr(wout_name, [D, D], BF16)
        wgu_d = self.dr(f"wgu{l}_bf", [D, 2 * DFF], BF16) if ffn else None
        wd_d = self.dr(f"wd{l}_bf", [DFF, D], BF16) if ffn else None
        ot_toks = self.all_toks(ot_name)
        wout = b.sb("wout", [128, 8, D], BF16); t_wout = Tok()
        b.dma("sp", wout[:], wout_d.rearrange("(c p) n -> p c n", p=128),
              reads=[self.dt_(wout_name, i) for i in range(4)], writes=[t_wout])
        wd = b.sb("wd", [128, 22, D], BF16); t_wd = Tok()
        for h in range(2 if ffn else 0):
            b.dma("sp", wd[:, h * 11:(h + 1) * 11, :], wd_d[h * 1408:(h + 1) * 1408, :].rearrange("(c p) n -> p c n", p=128),
                  reads=[self.dt_(f"wd{l}_bf", i) for i in range(11)], writes=[t_wd])
        R = self.norm_rings()
        xs_r = Ring(b, "xs", [128, 4, D], F32, 1)
        ot_r = Ring(b, "ot", [128, 8, G], BF16, 2)
        hT_r = Ring(b, "hT", [128, 8, G], BF16, 1)
        aT_r = Ring(b, "aT", [128, 22, G], BF16, 1)
        wg_r = Ring(b, "wg", [128, 8, 256], BF16, 3)
        wu_r = Ring(b, "wu", [128, 8, 256], BF16, 3)
        sg_r = Ring(b, "sg", [128, G], F32, 2)
        pacc = Ring(b, "pacc", [128, 512], F32, 2, psum=True)
        pg_r = Ring(b, "pg", [128, 512], F32, 2, psum=True)
        pu_r = Ring(b, "pu", [128, 512], F32, 2, psum=True)
        wgu_toks = [self.dt_(f"wgu{l}_bf", i) for i in range(4)] if ffn else []
        if final:
            out_d = self.dr(out_name, [TC, D], F32)
            yo_r = Ring(b, "yo", [128, D], F32, 2)
        for gi in range(NG):
            t0 = gi * G
            xs, t_xs = xs_r.next()
            b.dma("sp", xs[:], xin[t0:t0 + G, :].rearrange("(j p) d -> p j d", p=128), reads=[self.dt_(x_in, gi)], writes=[t_xs])
            ot, t_ot = ot_r.next()
            b.dma("sp", ot[:], ot_d[:, :, t0:t0 + G].rearrange("c p t -> p c t"), reads=(ot_toks or [self.dt_(ot_name, gi)]), writes=[t_ot])
            for j in range(4):
                for nh in range(2):
                    pa, t_pa = pacc.next()
                    for c in range(8):
                        b.op("pe", lambda c=c: nc.tensor.matmul(pa[:], lhsT=ot[:, c, j * 128:(j + 1) * 128],
                                                                rhs=wout[:, c, nh * 512:(nh + 1) * 512], start=(c == 0), stop=(c == 7)),
                             reads=[t_ot, t_wout], writes=[t_pa])
                    b.op("dve", lambda: nc.vector.tensor_tensor(out=xs[:, j, nh * 512:(nh + 1) * 512], in0=xs[:, j, nh * 512:(nh + 1) * 512],
                                                                in1=pa[:], op=ALU.add), reads=[t_pa, t_xs], writes=[t_xs])
            hT, t_hT = hT_r.next()
            for j in range(4 if ffn else 0):
                self.norm_tile(xs[:, j, :], t_xs, 4 + l, hT, t_hT, j * 128, R)
            aT, t_aT = aT_r.next()
            for s in range(11 if ffn else 0):
                wg, t_wg = wg_r.next()
                wu, t_wu = wu_r.next()
                b.dma("sp", wg[:], wgu_d[:, s * 256:(s + 1) * 256].rearrange("(c p) n -> p c n", p=128), reads=wgu_toks, writes=[t_wg])
                b.dma("sp", wu[:], wgu_d[:, DFF + s * 256:DFF + (s + 1) * 256].rearrange("(c p) n -> p c n", p=128), reads=wgu_toks, writes=[t_wu])
                for n2 in range(2):
                    n = s * 2 + n2
                    pg, t_pg = pg_r.next()
                    pu, t_pu = pu_r.next()
                    for k in range(8):
                        b.op("pe", lambda k=k: nc.tensor.matmul(pg[:], lhsT=wg[:, k, n2 * 128:(n2 + 1) * 128], rhs=hT[:, k, :],
                                                                start=(k == 0), stop=(k == 7)), reads=[t_wg, t_hT], writes=[t_pg])
                    for k in range(8):
                        b.op("pe", lambda k=k: nc.tensor.matmul(pu[:], lhsT=wu[:, k, n2 * 128:(n2 + 1) * 128], rhs=hT[:, k, :],
                                                                start=(k == 0), stop=(k == 7)), reads=[t_wu, t_hT], writes=[t_pu])
                    sg, t_sg = sg_r.next()
                    b.op("act", lambda: nc.scalar.activation(out=sg[:], in_=pg[:], func=AF.Silu), reads=[t_pg], writes=[t_sg])
                    b.op("dve", lambda: nc.vector.tensor_tensor(out=aT[:, n, :], in0=sg[:], in1=pu[:], op=ALU.mult),
                         reads=[t_sg, t_pu], writes=[t_aT])
            for j in range(4 if ffn else 0):
                for nh in range(2):
                    pa, t_pa = pacc.next()
                    for k in range(22):
                        b.op("pe", lambda k=k: nc.tensor.matmul(pa[:], lhsT=aT[:, k, j * 128:(j + 1) * 128],
                                                                rhs=wd[:, k, nh * 512:(nh + 1) * 512], start=(k == 0), stop=(k == 21)),
                             reads=[t_aT, t_wd], writes=[t_pa])
                    b.op("dve", lambda: nc.vector.tensor_tensor(out=xs[:, j, nh * 512:(nh + 1) * 512], in0=xs[:, j, nh * 512:(nh + 1) * 512],
                                                                in1=pa[:], op=ALU.add), reads=[t_pa, t_xs], writes=[t_xs])
            if not final:
                b.dma("pool", xout[t0:t0 + G, :].rearrange("(j p) d -> p j d", p=128), xs[:], reads=[t_xs],
                      writes=[self.dt_(x_out, gi)], is_output=(x_out in self.ext_out))
                if x_out2:
                    b.dma("pool", self.dr(x_out2, [TC, D], F32)[t0:t0 + G, :].rearrange("(j p) d -> p j d", p=128), xs[:], reads=[t_xs],
                          writes=[self.dt_(x_out2, gi)], is_output=True)
            else:
                for j in range(4):
                    junk, t_j = R["junk"].next()
                    ss, t_ss = R["ss"].next()
                    b.op("act", lambda: nc.scalar.activation(out=junk[:], in_=xs[:, j, :], func=AF.Square, accum_out=ss[:, 0:1]),
                         reads=[t_xs], writes=[t_j, t_ss])
                    b.op("act", lambda: nc.scalar.activation(out=ss[:, 1:2], in_=ss[:, 0:1], func=AF.Sqrt, scale=1.0 / D, bias=1e-6),
                         reads=[t_ss], writes=[t_ss])
                    b.op("dve", lambda: nc.vector.reciprocal(out=ss[:, 2:3], in_=ss[:, 1:2]), reads=[t_ss], writes=[t_ss])
                    yo, t_yo = yo_r.next()
                    b.op("dve", lambda: nc.vector.scalar_tensor_tensor(out=yo[:], in0=xs[:, j, :], scalar=ss[:, 2:3], in1=self.gains[:, 8, :],
                                                                       op0=ALU.mult, op1=ALU.mult), reads=[t_xs, t_ss, self.t_gains], writes=[t_yo])
                    b.dma("pool", out_d[t0 + j * 128:t0 + (j + 1) * 128, :], yo[:], reads=[t_yo], writes=[self.dt_(out_name, gi * 4 + j)],
                          is_output=True)
        b.pop()


def _attn_proj(self, l, x_in, win_name, KQ, aug0, halo_name=None, moba=False):
    b, nc = self.b, self.nc
    b.push()
    TK = TC + (HALO if halo_name else 0)
    xin = self.dr(x_in, [TC, D], F32)
    QT = self.dr(f"QT{l}", [4, 64 if moba else KQ, 4, TC], BF16)
    KT = self.dr(f"KT{l}", [4, 64 if moba else KQ, TK], BF16)
    V = self.dr(f"V{l}", [TK, 256], BF16)
    qaug = None if moba else self.dr(f"c_qaug", [4, 7, 4, TC], BF16)
    kaug = None if moba else self.dr(f"c_kaug{l}", [7, TK], BF16)
    win_d = self.dr(win_name, [D, 1536], BF16)
    win = b.sb("win", [128, 8, 1536], BF16); t_win = Tok()
    for h in range(3):
        b.dma("sp", win[:, :, h * 512:(h + 1) * 512], win_d[:, h * 512:(h + 1) * 512].rearrange("(c p) n -> p c n", p=128),
              reads=[self.dt_(win_name, i) for i in range(4)], writes=[t_win])
    for kvh in range(0 if moba else 4):
        b.dma("pool", QT[kvh, aug0:aug0 + 7, :, :], qaug[kvh], writes=[self.dt_(f"QT{l}", ("aug", kvh))])
        b.dma("pool", KT[kvh, aug0:aug0 + 7, :], kaug[:, :], writes=[self.dt_(f"KT{l}", ("aug", kvh))])
    if moba:
        KM = self.dr("KM", [4, 64, 128], F32)
        km_sb = b.sb("km_sb", [128, 2, 128], F32); t_km = Tok()
        kmj = b.sb("kmj", [128, 256], BF16); t_kmj = Tok()
        b.op("dve", lambda: nc.vector.memset(km_sb[:], 0.0), writes=[t_km])
    R = self.norm_rings()
    xs_r = Ring(b, "xs", [128, 4, D], F32, 2)
    hT_r = Ring(b, "hT", [128, 8, G], BF16, 2)
    pp = Ring(b, "pp", [128, 512], F32, 3, psum=True)
    st_r = Ring(b, "stg", [128, 512], BF16, 3)
    groups = ([(-1, HALO)] if halo_name else []) + [(gi, G) for gi in range(NG)]
    for gi, gn in groups:
        nj = gn // 128
        xs, t_xs = xs_r.next()
        if gi < 0:
            hx = self.dr(halo_name, [HALO, D], F32)
            b.dma("sp", xs[:, 0, :], hx[:, :], writes=[t_xs])
            tk0 = 0
            t0 = None
        else:
            t0 = gi * G
            tk0 = t0 + (HALO if halo_name else 0)
            b.dma("sp", xs[:], xin[t0:t0 + G, :].rearrange("(j p) d -> p j d", p=128), reads=[self.dt_(x_in, gi)], writes=[t_xs])
        hT, t_hT = hT_r.next()
        for j in range(nj):
            self.norm_tile(xs[:, j, :], t_xs, l, hT, t_hT, j * 128, R)
        gk = ("g", gi)
        if gi >= 0:
            for c in range(8):
                pq, t_pq = pp.next()
                for k in range(8):
                    b.op("pe", lambda k=k: nc.tensor.matmul(pq[:, :gn], lhsT=win[:, k, c * 128:(c + 1) * 128], rhs=hT[:, k, :gn],
                                                            start=(k == 0), stop=(k == 7)), reads=[t_win, t_hT], writes=[t_pq])
                st, t_st = st_r.next()
                b.op("act", lambda: nc.scalar.activation(out=st[:, :gn], in_=pq[:, :gn], func=AF.Copy, scale=0.125), reads=[t_pq], writes=[t_st])
                kvh = c // 2
                for hh in range(2):
                    g = 2 * (c % 2) + hh
                    b.dma("pool", QT[kvh, 0:64, g, t0:t0 + gn], st[hh * 64:(hh + 1) * 64, :gn], reads=[t_st],
                          writes=[self.dt_(f"QT{l}", (gi, kvh, g))], is_output=(f"QT{l}" in self.ext_out))
        for c in range(2):
            pk, t_pk = pp.next()
            for k in range(8):
                b.op("pe", lambda k=k: nc.tensor.matmul(pk[:, :gn], lhsT=win[:, k, 1024 + c * 128:1024 + (c + 1) * 128], rhs=hT[:, k, :gn],
                                                        start=(k == 0), stop=(k == 7)), reads=[t_win, t_hT], writes=[t_pk])
            st, t_st = st_r.next()
            b.op("act", lambda: nc.scalar.copy(out=st[:, :gn], in_=pk[:, :gn]), reads=[t_pk], writes=[t_st])
            for hh in range(2):
                kvh = 2 * c + hh
                b.dma("pool", KT[kvh, 0:64, tk0:tk0 + gn], st[hh * 64:(hh + 1) * 64, :gn], reads=[t_st],
                      writes=[self.dt_(f"KT{l}", (gi, kvh))], is_output=(f"KT{l}" in self.ext_out))
            if moba:
                for a2 in range(2):
                    b.op("act", lambda: nc.scalar.activation(out=kmj[:], in_=pk[:, a2 * 256:(a2 + 1) * 256], func=AF.Copy,
                                                             accum_out=km_sb[:, c, gi * 2 + a2:gi * 2 + a2 + 1]),
                         reads=[t_pk], writes=[t_km, t_kmj])
        for j in range(nj):
            pv, t_pv = pp.next()
            for k in range(8):
                b.op("pe", lambda k=k: nc.tensor.matmul(pv[:, 0:256], lhsT=hT[:, k, j * 128:(j + 1) * 128], rhs=win[:, k, 1280:1536],
                                                        start=(k == 0), stop=(k == 7)), reads=[t_win, t_hT], writes=[t_pv])
            st, t_st = st_r.next()
            b.op("act", lambda: nc.scalar.copy(out=st[:, 0:256], in_=pv[:, 0:256]), reads=[t_pv], writes=[t_st])
            b.dma("pool", V[tk0 + j * 128:tk0 + (j + 1) * 128, :], st[:, 0:256], reads=[t_st], writes=[self.dt_(f"V{l}", (gi, j))])
    if moba:
        b.op("dve", lambda: nc.vector.tensor_scalar(out=km_sb[:], in0=km_sb[:], scalar1=1.0 / 256, scalar2=None, op0=ALU.mult),
             reads=[t_km], writes=[t_km])
        for c in range(2):
            b.dma("pool", KM[2 * c:2 * c + 2].rearrange("h d n -> (h d) n"), km_sb[:, c, :], reads=[t_km], writes=[self.dt_("KM", c)],
                  is_output=("KM" in self.ext_out))
    b.pop()


Prog.attn_proj = _attn_proj


def _all_toks(self, name):
    return [t for (n, i), t in self.dtoks.items() if n == name]


Prog.all_toks = _all_toks


def _stage_swa(self):
    b, nc = self.b, self.nc
    b.push()
    KQ = 71
    TK = TC + HALO
    QT = self.dr("QT0", [4, KQ, 4, TC], BF16)
    KT = self.dr("KT0", [4, KQ, TK], BF16)
    V = self.dr("V0", [TK, 256], BF16)
    OT = self.dr("OT0", [8, 128, TC], BF16)
    msk_d = self.dr("c_swamask", [3, 128, 512], BF16)
    sinks_d = self.dr("swa_sinks", [1, 16], F32)
    msk = b.sb("msk", [128, 3, 512], BF16); t_msk = Tok()
    b.dma("sp", msk[:], msk_d.rearrange("m p n -> p m n"), writes=[t_msk])
    snk = b.sb("snk", [1, 16], F32); t_snk = Tok()
    b.dma("sp", snk[:], sinks_d[:, :], writes=[t_snk])
    b.op("act", lambda: nc.scalar.activation(out=snk[:], in_=snk[:], func=AF.Exp), reads=[t_snk], writes=[t_snk])
    es = b.sb("es", [1, 16, 128], BF16); t_es = Tok()
    for h in range(16):
        b.op("dve", lambda h=h: nc.vector.tensor_scalar(out=es[0:1, h, :], in0=self.ones[0:1, 0:128], scalar1=snk[0:1, h:h + 1], scalar2=None,
                                                         op0=ALU.mult), reads=[t_snk, self.t_ones], writes=[t_es])
    kt_r = Ring(b, "kt", [KQ, TK], BF16, 2)
    v_r = Ring(b, "v", [128, TK // 128, 64], BF16, 2)
    qt_r = Ring(b, "qt", [KQ, 512], BF16, 3)
    pT_r = Ring(b, "pT", [128, 512], BF16, 3)
    ps_r = Ring(b, "pss", [128, 512], F32, 2, psum=True)
    pn_r = Ring(b, "psn", [128, 512], F32, 2, psum=True)
    pd_r = Ring(b, "psd", [128, 512], F32, 2, psum=True)
    rc_r = Ring(b, "rc", [128, 256], F32, 2)
    ot_r = Ring(b, "oto", [128, 2, TC], BF16, 2)
    qt_toks = self.all_toks("QT0")
    kt_toks = self.all_toks("KT0")
    v_toks = self.all_toks("V0")
    for kvh in range(4):
        kt, t_kt = kt_r.next()
        b.dma("sp", kt[:], KT[kvh], reads=kt_toks, writes=[t_kt])
        v, t_v = v_r.next()
        b.dma("sp", v[:], V[:, kvh * 64:(kvh + 1) * 64].rearrange("(n p) d -> p n d", p=128), reads=v_toks, writes=[t_v])
        oto, t_oto = ot_r.next()
        for qb in range(NT):
            qt, t_qt = qt_r.next()
            b.dma("sp", qt[:].rearrange("k (g t) -> k g t", g=4), QT[kvh, :, :, qb * 128:(qb + 1) * 128], reads=qt_toks, writes=[t_qt])
            pn, t_pn = pn_r.next()
            pd, t_pd = pd_r.next()
            for ki, (ktile, mi) in enumerate(((qb, 0 if qb == 0 else 1), (qb + 1, 2))):
                ps, t_ps = ps_r.next()
                b.op("pe", lambda: nc.tensor.matmul(ps[:], lhsT=kt[:, ktile * 128:(ktile + 1) * 128], rhs=qt[:], start=True, stop=False),
                     reads=[t_kt, t_qt], writes=[t_ps])
                b.op("pe", lambda: nc.tensor.matmul(ps[:], lhsT=self.ident[:], rhs=msk[:, mi, :], start=False, stop=True),
                     reads=[self.t_ident, t_msk], writes=[t_ps])
                pT, t_pT = pT_r.next()
                b.op("act", lambda: nc.scalar.activation(out=pT[:], in_=ps[:], func=AF.Exp), reads=[t_ps], writes=[t_pT])
                for g in range(4):
                    po = (g % 2) * 64
                    co = (g // 2) * 128
                    b.op("pe", lambda: nc.tensor.matmul(pn[po:po + 64, co:co + 128], lhsT=v[:, ktile, :], rhs=pT[:, g * 128:(g + 1) * 128],
                                                        start=(ki == 0 and g < 2), stop=(ki == 1), tile_position=(0, po)),
                         reads=[t_v, t_pT], writes=[t_pn])
                    b.op("pe", lambda: nc.tensor.matmul(pd[po:po + 64, co:co + 128], lhsT=self.ones[:, 0:64], rhs=pT[:, g * 128:(g + 1) * 128],
                                                        start=(ki == 0 and g < 2), stop=False, tile_position=(0, po)),
                         reads=[self.t_ones, t_pT], writes=[t_pd])
            for g in range(4):
                po = (g % 2) * 64
                co = (g // 2) * 128
                b.op("pe", lambda: nc.tensor.matmul(pd[po:po + 64, co:co + 128], lhsT=self.ones[0:1, 0:64], rhs=es[0:1, kvh * 4 + g, :],
                                                    start=False, stop=True, tile_position=(0, po)),
                     reads=[self.t_ones, t_es], writes=[t_pd])
            rc, t_rc = rc_r.next()
            b.op("dve", lambda: nc.vector.reciprocal(out=rc[:], in_=pd[:, 0:256]), reads=[t_pd], writes=[t_rc])
            b.op("dve", lambda: nc.vector.tensor_tensor(out=oto[:, :, qb * 128:(qb + 1) * 128], in0=pn[:, 0:256].rearrange("p (c t) -> p c t", c=2),
                                                        in1=rc[:].rearrange("p (c t) -> p c t", c=2), op=ALU.mult),
                 reads=[t_pn, t_rc], writes=[t_oto])
        for cc in range(2):
            b.dma("pool", OT[2 * kvh + cc], oto[:, cc, :], reads=[t_oto], writes=[self.dt_("OT0", ("c", 2 * kvh + cc))])
            if "dbg_ot" in self.ext_out:
                b.dma("pool", self.dr("dbg_ot", [8, 128, TC], BF16)[2 * kvh + cc], oto[:, cc, :], reads=[t_oto], writes=[self.dt_("dbg_ot", 2 * kvh + cc)])
    b.pop()


Prog.stage_swa = _stage_swa


def _stage_moba(self):
    b, nc = self.b, self.nc
    b.push()
    KQ = 103
    NB = 64
    TKS = NB * 256
    QT = self.dr("QT1", [4, 64, 4, TC], BF16)
    qaug = self.dr("c_qaug", [4, 7, 4, TC], BF16)
    GKT = self.dr("GKT", [4, 64, TKS], BF16)
    GV = self.dr("GV", [TKS, 256], BF16)
    GKM = self.dr("GKM", [4, 64, NB], F32)
    OT = self.dr("OT1", [8, 128, TC], BF16)
    kaug = self.dr("c_kaug1", [7, TKS], BF16)
    oneh = self.dr("c_onehot", [32, TKS], BF16)
    valid_d = self.dr("c_valid", [TC, NB], F32)
    cm_d = self.dr("c_swamask", [3, 128, 512], BF16)
    cmask = b.sb("cmask", [128, 512], BF16); t_cm = Tok()
    b.dma("sp", cmask[:], cm_d[2], writes=[t_cm])
    kt = b.sb("kt", [KQ, TKS], BF16); t_kt = Tok()
    b.dma("sp", kt[64:96, :], oneh[:, :], writes=[t_kt])
    b.dma("sp", kt[96:103, :], kaug[:, :], writes=[t_kt])
    v = b.sb("v", [128, TKS // 128, 64], BF16); t_v = Tok()
    km = b.sb("km", [64, NB], F32); t_km = Tok()
    kmh = b.sb("kmh", [64, 2, NB], BF16); t_kmh = Tok()
    kmr = b.sb("kmr", [64, NB], F32)
    mv_r = Ring(b, "mv", [128, 2, 128], BF16, 2)
    for mvb in mv_r.bufs:
        b.op("dve", lambda mvb=mvb: nc.vector.memset(mvb[:], 0.0), writes=mv_r.toks)
    qz_r = Ring(b, "qz", [KQ, 512], BF16, 2)
    ql_r = Ring(b, "ql", [KQ, 512], BF16, 2)
    qh_r = Ring(b, "qh", [KQ, 512], BF16, 2)
    for qzb, tqz in zip(qz_r.bufs, qz_r.toks):
        b.op("dve", lambda qzb=qzb: nc.vector.memset(qzb[64:96, :], 0.0), writes=[tqz])
    qs_r = Ring(b, "qs", [64, 128], F32, 2)
    qsb_r = Ring(b, "qsb", [64, 2, 128], BF16, 2)
    qsr_r = Ring(b, "qsr", [64, 128], F32, 2)
    val_r = Ring(b, "val", [128, NB], F32, 2)
    gm_r = Ring(b, "gm", [128, NB], F32, 2)
    t8_r = Ring(b, "t8", [128, 8], F32, 2)
    pT_r = Ring(b, "pT", [128, 512], BF16, 3)
    ps_r = Ring(b, "pss", [128, 512], F32, 2, psum=True)
    pn_r = Ring(b, "psn", [128, 512], F32, 1, psum=True)
    pd_r = Ring(b, "psd", [128, 512], F32, 1, psum=True)
    pg_r = Ring(b, "psg", [128, 512], F32, 1, psum=True)
    pm_r = Ring(b, "psm", [128, 1024], BF16, 1, psum=True)
    rc_r = Ring(b, "rc", [128, 256], F32, 2)
    ot_r = Ring(b, "oto", [128, 2, TC], BF16, 2)
    qt_toks = self.all_toks("QT1") or [self.dt_("QT1")]
    gk_toks = self.all_toks("GKT") or [self.dt_("GKT")]
    gv_toks = self.all_toks("GV") or [self.dt_("GV")]
    gm_toks = self.all_toks("GKM") or [self.dt_("GKM")]
    for kvh in range(4):
        b.dma("sp", kt[0:64, :], GKT[kvh], reads=gk_toks, writes=[t_kt])
        b.dma("sp", v[:], GV[:, kvh * 64:(kvh + 1) * 64].rearrange("(n p) d -> p n d", p=128), reads=gv_toks, writes=[t_v])
        b.dma("sp", km[:], GKM[kvh], reads=gm_toks, writes=[t_km])
        b.op("dve", lambda: nc.vector.tensor_copy(out=kmh[:, 0, :], in_=km[:]), reads=[t_km], writes=[t_kmh])
        b.op("dve", lambda: nc.vector.tensor_tensor(out=kmr[:], in0=km[:], in1=kmh[:, 0, :], op=ALU.subtract), reads=[t_km, t_kmh], writes=[t_kmh])
        b.op("dve", lambda: nc.vector.tensor_copy(out=kmh[:, 1, :], in_=kmr[:]), reads=[t_kmh], writes=[t_kmh])
        oto, t_oto = ot_r.next()
        for qi in range(NT):
            qsl = slice(qi * 128, (qi + 1) * 128)
            qz, t_qz = qz_r.next(); ql, t_ql = ql_r.next(); qh, t_qh = qh_r.next()
            for qq, tq in ((qz, t_qz), (ql, t_ql), (qh, t_qh)):
                b.dma("sp", qq[0:64, :].rearrange("k (g t) -> k g t", g=4), QT[kvh, :, :, qsl], reads=qt_toks, writes=[tq])
                b.dma("sp", qq[96:103, :].rearrange("k (g t) -> k g t", g=4), qaug[kvh, :, :, qsl], writes=[tq])
            val, t_val = val_r.next()
            b.dma("sp", val[:], valid_d[qsl, :], writes=[t_val])
            qs, t_qs = qs_r.next(); qsb, t_qsb = qsb_r.next(); qsr, t_qsr = qsr_r.next()
            b.op("dve", lambda: nc.vector.tensor_tensor(out=qs[:], in0=qz[0:64, 0:128], in1=qz[0:64, 128:256], op=ALU.add), reads=[t_qz], writes=[t_qs])
            b.op("dve", lambda: nc.vector.tensor_tensor(out=qs[:], in0=qs[:], in1=qz[0:64, 256:384], op=ALU.add), reads=[t_qz, t_qs], writes=[t_qs])
            b.op("dve", lambda: nc.vector.tensor_tensor(out=qs[:], in0=qs[:], in1=qz[0:64, 384:512], op=ALU.add), reads=[t_qz, t_qs], writes=[t_qs])
            b.op("dve", lambda: nc.vector.tensor_copy(out=qsb[:, 0, :], in_=qs[:]), reads=[t_qs], writes=[t_qsb])
            b.op("dve", lambda: nc.vector.tensor_tensor(out=qsr[:], in0=qs[:], in1=qsb[:, 0, :], op=ALU.subtract), reads=[t_qs, t_qsb], writes=[t_qsr])
            b.op("dve", lambda: nc.vector.tensor_copy(out=qsb[:, 1, :], in_=qsr[:]), reads=[t_qsr], writes=[t_qsb])
            pg, t_pg = pg_r.next()
            for i3, (a_, c_) in enumerate(((0, 0), (0, 1), (1, 0))):
                b.op("pe", lambda: nc.tensor.matmul(pg[:, 0:NB], lhsT=qsb[:, a_, :], rhs=kmh[:, c_, :], start=(i3 == 0), stop=(i3 == 2)),
                     reads=[t_qsb, t_kmh], writes=[t_pg])
            gm, t_gm = gm_r.next(); t8, t_t8 = t8_r.next()
            b.op("dve", lambda: nc.vector.tensor_tensor(out=gm[:], in0=pg[:, 0:NB], in1=val[:], op=ALU.add), reads=[t_pg, t_val], writes=[t_gm])
            b.op("dve", lambda: nc.vector.max(out=t8[:], in_=gm[:]), reads=[t_gm], writes=[t_t8])
            b.op("dve", lambda: nc.vector.tensor_scalar(out=gm[:], in0=gm[:], scalar1=t8[:, 2:3], scalar2=1.0, op0=ALU.is_ge, op1=ALU.subtract),
                 reads=[t_gm, t_t8], writes=[t_gm])
            mv, t_mv = mv_r.next()
            b.op("dve", lambda: nc.vector.scalar_tensor_tensor(out=mv[:, :, 64:96], in0=gm[:].rearrange("p (h r) -> p h r", h=2), scalar=-NEG,
                                                               in1=val[:].rearrange("p (h r) -> p h r", h=2), op0=ALU.mult, op1=ALU.add),
                 reads=[t_gm, t_val], writes=[t_mv])
            pm, t_pm = pm_r.next()
            for hf in range(2):
                b.op("pe", lambda: nc.tensor.transpose(pm[:, hf * 128:(hf + 1) * 128], mv[:, hf, :], self.ident[:]),
                     reads=[t_mv, self.t_ident], writes=[t_pm])
            for hf, (qq, tq) in enumerate(((ql, t_ql), (qh, t_qh))):
                for g in range(4):
                    b.op("act", lambda: nc.scalar.copy(out=qq[64:96, g * 128:(g + 1) * 128], in_=pm[64:96, hf * 128:(hf + 1) * 128]),
                         reads=[t_pm], writes=[tq])
            bi, par = qi // 2, qi % 2
            tiles = []
            for rho in range(48 + bi):
                for hh in range(2):
                    tiles.append((rho * 2 + hh, (ql, t_ql) if rho < 32 else (qh, t_qh), False))
            own = 48 + bi
            if par == 0:
                tiles.append((own * 2, (qz, t_qz), True))
            else:
                tiles.append((own * 2, (qz, t_qz), False))
                tiles.append((own * 2 + 1, (qz, t_qz), True))
            pn, t_pn = pn_r.next()
            pd, t_pd = pd_r.next()
            nt = len(tiles)
            for ti, (ktile, (qq, tq), tri) in enumerate(tiles):
                ps, t_ps = ps_r.next()
                b.op("pe", lambda: nc.tensor.matmul(ps[:], lhsT=kt[:, ktile * 128:(ktile + 1) * 128], rhs=qq[:], start=True, stop=not tri),
                     reads=[t_kt, tq], writes=[t_ps])
                if tri:
                    b.op("pe", lambda: nc.tensor.matmul(ps[:], lhsT=self.ident[:], rhs=cmask[:], start=False, stop=True),
                         reads=[self.t_ident, t_cm], writes=[t_ps])
                pT, t_pT = pT_r.next()
                b.op("act", lambda: nc.scalar.activation(out=pT[:], in_=ps[:], func=AF.Exp), reads=[t_ps], writes=[t_pT])
                for g in range(4):
                    po = (g % 2) * 64
                    co = (g // 2) * 128
                    b.op("pe", lambda: nc.tensor.matmul(pn[po:po + 64, co:co + 128], lhsT=v[:, ktile, :], rhs=pT[:, g * 128:(g + 1) * 128],
                                                        start=(ti == 0 and g < 2), stop=(ti == nt - 1), tile_position=(0, po)),
                         reads=[t_v, t_pT], writes=[t_pn])
                    b.op("pe", lambda: nc.tensor.matmul(pd[po:po + 64, co:co + 128], lhsT=self.ones[:, 0:64], rhs=pT[:, g * 128:(g + 1) * 128],
                                                        start=(ti == 0 and g < 2), stop=(ti == nt - 1), tile_position=(0, po)),
                         reads=[self.t_ones, t_pT], writes=[t_pd])
            rc, t_rc = rc_r.next()
            b.op("dve", lambda: nc.vector.reciprocal(out=rc[:], in_=pd[:, 0:256]), reads=[t_pd], writes=[t_rc])
            b.op("dve", lambda: nc.vector.tensor_tensor(out=oto[:, :, qsl], in0=pn[:, 0:256].rearrange("p (c t) -> p c t", c=2),
                                                        in1=rc[:].rearrange("p (c t) -> p c t", c=2), op=ALU.mult),
                 reads=[t_pn, t_rc], writes=[t_oto])
        for cc in range(2):
            b.dma("pool", OT[2 * kvh + cc], oto[:, cc, :], reads=[t_oto], writes=[self.dt_("OT1", ("c", 2 * kvh + cc))])
    b.pop()


Prog.stage_moba = _stage_moba


def _lin_proj(self, l, kind, x_in):
    b, nc = self.b, self.nc
    b.push()
    gla = (kind == 2)
    H = 4 if gla else 8
    NIN = 3088 if gla else 4096
    sfx = str(l)
    xin = self.dr(x_in, [TC, D], F32)
    QG = self.dr("QG" + sfx, [H, 128, TC], BF16)
    KG = self.dr("KG" + sfx, [H, 128, TC], BF16)
    LG = self.dr("LG" + sfx, [H, 128, TC], F32)
    VG = self.dr("VG" + sfx, [TC, D], BF16)
    SG = self.dr("SG" + sfx, [8, 128, TC], BF16)
    wname = f"win{l}_bf"
    win_d = self.dr(wname, [D, NIN], BF16)
    win = b.sb("win", [128, 8, NIN], BF16); t_win = Tok()
    wt = [self.dt_(wname, i) for i in range(4)]
    for c0 in range(0, NIN, 512):
        c1 = min(NIN, c0 + 512)
        b.dma("sp", win[:, :, c0:c1], win_d[:, c0:c1].rearrange("(c p) n -> p c n", p=128), reads=wt, writes=[t_win])
    if gla:
        wup_d = self.dr("gla_w_decay_up", [1, 16, 512], F32)
        bd_d = self.dr("gla_b_decay", [1, 512], F32)
        wupf = b.sb("wupf", [16, 512], F32); t_wupf = Tok()
        b.dma("sp", wupf[:], wup_d[0], writes=[t_wupf])
        wup = b.sb("wup", [16, 512], BF16); t_wup = Tok()
        b.op("dve", lambda: nc.vector.tensor_copy(out=wup[:], in_=wupf[:]), reads=[t_wupf], writes=[t_wup])
        nb = b.sb("nb", [128, 4], F32); t_nb = Tok()
        with nc.allow_non_contiguous_dma(reason="tiny bias"):
            b.dma("sp", nb[:], bd_d[0].rearrange("(h p) -> p h", p=128), writes=[t_nb])
        b.op("dve", lambda: nc.vector.tensor_scalar(out=nb[:], in0=nb[:], scalar1=-1.0, scalar2=None, op0=ALU.mult), reads=[t_nb], writes=[t_nb])
        aT_r = Ring(b, "aT", [16, G], BF16, 2)
    else:
        lbl_d = self.dr("hgrn_lb_logits", [4, D], F32)
        lg4 = b.sb("lg4", [128, 8, 4], F32); t_lg4 = Tok()
        with nc.allow_non_contiguous_dma(reason="tiny table"):
            for j in range(4):
                b.dma("sp", lg4[:, :, j], lbl_d[j].rearrange("(h p) -> p h", p=128), writes=[t_lg4])
        b.op("act", lambda: nc.scalar.activation(out=lg4[:], in_=lg4[:], func=AF.Exp), reads=[t_lg4], writes=[t_lg4])
        lbs = b.sb("lbs", [128, 8, 4], F32); t_lb = Tok()
        b.op("dve", lambda: nc.vector.tensor_reduce(out=lbs[:, :, 0], in_=lg4[:], axis=AX.X, op=ALU.add), reads=[t_lg4], writes=[t_lb])
        b.op("dve", lambda: nc.vector.tensor_reduce(out=lbs[:, :, 1], in_=lg4[:, :, 1:l + 1], axis=AX.X, op=ALU.add), reads=[t_lg4], writes=[t_lb])
        b.op("dve", lambda: nc.vector.reciprocal(out=lbs[:, :, 0], in_=lbs[:, :, 0]), reads=[t_lb], writes=[t_lb])
        b.op("dve", lambda: nc.vector.tensor_tensor(out=lbs[:, :, 2], in0=lbs[:, :, 1], in1=lbs[:, :, 0], op=ALU.mult), reads=[t_lb], writes=[t_lb])
        b.op("dve", lambda: nc.vector.tensor_scalar(out=lbs[:, :, 3], in0=lbs[:, :, 2], scalar1=-1.0, scalar2=1.0, op0=ALU.mult, op1=ALU.add),
             reads=[t_lb], writes=[t_lb])
    R = self.norm_rings()
    xs_r = Ring(b, "xs", [128, 4, D], F32, 2)
    hT_r = Ring(b, "hT", [128, 8, G], BF16, 2)
    pp = Ring(b, "pp", [128, 512], F32, 3, psum=True)
    sb_r = Ring(b, "stb", [128, 512], BF16, 3)
    sf_r = Ring(b, "stf", [128, 512], F32, 3)
    sf2_r = Ring(b, "stf2", [128, 512], F32, 2)
    qscale = 128.0 ** -0.5

    def fm_mm(col0, hT, t_hT, M=128):
        pq, t_pq = pp.next()
        for k in range(8):
            b.op("pe", lambda k=k: nc.tensor.matmul(pq[0:M, :], lhsT=win[:, k, col0:col0 + M], rhs=hT[:, k, :], start=(k == 0), stop=(k == 7)),
                 reads=[t_win, t_hT], writes=[t_pq])
        return pq, t_pq

    for gi in range(NG):
        t0 = gi * G
        xs, t_xs = xs_r.next()
        b.dma("sp", xs[:], xin[t0:t0 + G, :].rearrange("(j p) d -> p j d", p=128), reads=[self.dt_(x_in, gi)], writes=[t_xs])
        hT, t_hT = hT_r.next()
        for j in range(4):
            self.norm_tile(xs[:, j, :], t_xs, l, hT, t_hT, j * 128, R)
        for h in range(H):
            pq, t_pq = fm_mm(h * 128, hT, t_hT)
            st, t_st = sb_r.next()
            if gla:
                b.op("act", lambda: nc.scalar.activation(out=st[:], in_=pq[:], func=AF.Copy, scale=qscale), reads=[t_pq], writes=[t_st])
            else:
                sf, t_sf = sf_r.next()
                b.op("act", lambda: nc.scalar.activation(out=sf[:], in_=pq[:], func=AF.Silu), reads=[t_pq], writes=[t_sf])
                b.op("dve", lambda: nc.vector.tensor_scalar(out=st[:], in0=sf[:], scalar1=qscale, scalar2=None, op0=ALU.mult), reads=[t_sf], writes=[t_st])
            b.dma("pool", QG[h, :, t0:t0 + G], st[:], reads=[t_st], writes=[self.dt_("QG" + sfx, (gi, h))])
            if gla:
                pk, t_pk = fm_mm(512 + h * 128, hT, t_hT)
                st, t_st = sb_r.next()
                b.op("act", lambda: nc.scalar.copy(out=st[:], in_=pk[:]), reads=[t_pk], writes=[t_st])
                b.dma("pool", KG[h, :, t0:t0 + G], st[:], reads=[t_st], writes=[self.dt_("KG" + sfx, (gi, h))])
            else:
                pf, t_pf = fm_mm(1024 + h * 128, hT, t_hT)
                sf, t_sf = sf_r.next()
                b.op("act", lambda: nc.scalar.activation(out=sf[:], in_=pf[:], func=AF.Sigmoid), reads=[t_pf], writes=[t_sf])
                b.op("dve", lambda: nc.vector.tensor_scalar(out=sf[:], in0=sf[:], scalar1=lbs[:, h, 3:4], scalar2=lbs[:, h, 2:3], op0=ALU.mult, op1=ALU.add),
                     reads=[t_sf, t_lb], writes=[t_sf])
                st, t_st = sb_r.next()
                b.op("dve", lambda: nc.vector.tensor_scalar(out=st[:], in0=sf[:], scalar1=-1.0, scalar2=1.0, op0=ALU.mult, op1=ALU.add),
                     reads=[t_sf], writes=[t_st])
                b.dma("pool", KG[h, :, t0:t0 + G], st[:], reads=[t_st], writes=[self.dt_("KG" + sfx, (gi, h))])
                s2, t_s2 = sf2_r.next()
                b.op("act", lambda: nc.scalar.activation(out=s2[:], in_=sf[:], func=AF.Ln), reads=[t_sf], writes=[t_s2])
                b.dma("pool", LG[h, :, t0:t0 + G], s2[:], reads=[t_s2], writes=[self.dt_("LG" + sfx, (gi, h))])
        if gla:
            pa_, t_pa = fm_mm(3072, hT, t_hT, M=16)
            aT, t_aT = aT_r.next()
            b.op("act", lambda: nc.scalar.copy(out=aT[:], in_=pa_[0:16, :]), reads=[t_pa], writes=[t_aT])
            for h in range(4):
                pz, t_pz = pp.next()
                b.op("pe", lambda: nc.tensor.matmul(pz[:], lhsT=wup[0:16, h * 128:(h + 1) * 128], rhs=aT[0:16, :], start=True, stop=True),
                     reads=[t_wup, t_aT], writes=[t_pz])
                sf, t_sf = sf_r.next()
                b.op("act", lambda: nc.scalar.activation(out=sf[:], in_=pz[:], func=AF.Exp, scale=-1.0, bias=nb[:, h:h + 1]),
                     reads=[t_pz, t_nb], writes=[t_sf])
                b.op("act", lambda: nc.scalar.activation(out=sf[:], in_=sf[:], func=AF.Ln, bias=1.0), reads=[t_sf], writes=[t_sf])
                s2, t_s2 = sf2_r.next()
                b.op("dve", lambda: nc.vector.tensor_scalar(out=s2[:], in0=sf[:], scalar1=-1.0 / 16.0, scalar2=None, op0=ALU.mult), reads=[t_sf], writes=[t_s2])
                b.dma("pool", LG[h, :, t0:t0 + G], s2[:], reads=[t_s2], writes=[self.dt_("LG" + sfx, (gi, h))])
        gcol = 2048 if gla else 3072
        for c in range(8):
            pg, t_pg = fm_mm(gcol + c * 128, hT, t_hT)
            st, t_st = sb_r.next()
            b.op("act", lambda: nc.scalar.activation(out=st[:], in_=pg[:], func=AF.Silu), reads=[t_pg], writes=[t_st])
            b.dma("pool", SG[c, :, t0:t0 + G], st[:], reads=[t_st], writes=[self.dt_("SG" + sfx, (gi, c))])
        vcol = 1024 if gla else 2048
        for j in range(4):
            for nh in range(2):
                pv, t_pv = pp.next()
                for k in range(8):
                    b.op("pe", lambda k=k: nc.tensor.matmul(pv[:], lhsT=hT[:, k, j * 128:(j + 1) * 128], rhs=win[:, k, vcol + nh * 512:vcol + (nh + 1) * 512],
                                                            start=(k == 0), stop=(k == 7)), reads=[t_win, t_hT], writes=[t_pv])
                st, t_st = sb_r.next()
                b.op("act", lambda: nc.scalar.copy(out=st[:], in_=pv[:]), reads=[t_pv], writes=[t_st])
                b.dma("pool", VG[t0 + j * 128:t0 + (j + 1) * 128, nh * 512:(nh + 1) * 512], st[:], reads=[t_st], writes=[self.dt_("VG" + sfx, (gi, j, nh))])
    b.pop()


Prog.lin_proj = _lin_proj


def _lin_pass(self, l, kind, mode):
    b, nc = self.b, self.nc
    b.push()
    gla = (kind == 2)
    H = 4 if gla else 8
    V = 256 if gla else 128
    VT = V // 128
    sfx = str(l)
    GL = 256
    NB2 = 2 if gla else 1
    NGL = TC // GL
    NCH = GL // 64
    QG = self.dr("QG" + sfx, [H, 128, TC], BF16)
    KG = self.dr("KG" + sfx, [H, 128, TC], BF16)
    LG = self.dr("LG" + sfx, [H, 128, TC], F32)
    VG = self.dr("VG" + sfx, [TC, D], BF16)
    SG = self.dr("SG" + sfx, [8, 128, TC], BF16)
    tq, tk, tl, tv, tsg = [self.all_toks(n + sfx) for n in ("QG", "KG", "LG", "VG", "SG")]
    rmask = b.sb("rmask", [128, H, GL], F32); t_rm = Tok()
    b.op("dve", lambda: nc.vector.memset(rmask[:], 1.0), writes=[t_rm])
    b.op("dve", lambda: nc.vector.memset(rmask[:].rearrange("p h (c s) -> p h c s", s=64)[:, :, :, 0:1], 0.0), writes=[t_rm])
    tri_d = self.dr("c_tri", [128, 512], BF16)
    tri = b.sb("tri", [128, 512], BF16); t_tri = Tok()
    b.dma("sp", tri[:], tri_d[:, :], writes=[t_tri])
    S = b.sb("S", [128, H, V], F32); t_S = Tok()
    if mode == 1:
        b.op("dve", lambda: nc.vector.memset(S[:], 0.0), writes=[t_S])
        bsum = b.sb("bsum", [128, 128], F32); t_bs = Tok()
        b.op("dve", lambda: nc.vector.memset(bsum[:], 0.0), writes=[t_bs])
        bred = b.sb("bred", [128, H], F32)
    else:
        sp = b.sb("sp", [128, 3, H * V], F32); t_sp = Tok()
        dp = b.sb("dp", [128, 2, 128], F32); t_dp = Tok()
        for i in range(3):
            b.dma("sp", sp[:, i, :], self.dr(f"SP{i + 1}", [128, H * V], F32)[:, :], writes=[t_sp])
        for i in range(2):
            b.dma("sp", dp[:, i, :], self.dr(f"DP{i + 1}", [128, 128], F32)[:, :], writes=[t_dp])
        for h in range(H):
            hs = slice(h * V, (h + 1) * V)
            b.op("dve", lambda: nc.vector.scalar_tensor_tensor(out=S[:, h, :], in0=sp[:, 2, hs], scalar=dp[:, 1, h:h + 1], in1=sp[:, 1, hs],
                                                               op0=ALU.mult, op1=ALU.add), reads=[t_sp, t_dp], writes=[t_S])
            b.op("dve", lambda: nc.vector.scalar_tensor_tensor(out=S[:, h, :], in0=S[:, h, :], scalar=dp[:, 0, h:h + 1], in1=sp[:, 0, hs],
                                                               op0=ALU.mult, op1=ALU.add), reads=[t_sp, t_dp, t_S], writes=[t_S])
        sbf_r = Ring(b, "sbf", [128, H, V], BF16, 2)
        sbf, t_sbf = sbf_r.next()
        b.op("act", lambda: nc.scalar.copy(out=sbf[:], in_=S[:]), reads=[t_S], writes=[t_sbf])
        OT = self.dr("OT" + sfx, [8, 128, TC], BF16)
        wn_d = self.dr("gla_out_norm" if gla else "hgrn_out_norm", [1, V], F32)
        wn = b.sb("wn", [128, VT], F32); t_wn = Tok()
        with nc.allow_non_contiguous_dma(reason="tiny gain"):
            b.dma("sp", wn[:], wn_d[0].rearrange("(v p) -> p v", p=128), writes=[t_wn])
        q_r = Ring(b, "q", [128, H, GL], BF16, NB2)
        sg_r = Ring(b, "sg", [128, 8, GL], BF16, NB2)
        qd_r = Ring(b, "qd", [128, H, GL], BF16, NB2)
        kd_r = Ring(b, "kd", [128, H, GL], BF16, NB2)
        att_r = Ring(b, "att", [128, NCH // 2, H * 64], BF16, 2)
        oraw_r = Ring(b, "oraw", [128, 8, GL], F32, NB2)
        sq_r = Ring(b, "sq", [128, 8, GL], BF16, 1)
        rs_r = Ring(b, "rs", [128, GL], F32, 2)
        t1_r = Ring(b, "t1", [128, GL], F32, 2)
        ot_r = Ring(b, "otg", [128, 8, GL], BF16, NB2)
        pa_r = Ring(b, "pa", [128, 512], F32, 1, psum=True)
        po_r = Ring(b, "po", [128, 512], F32, 2, psum=True)
    lg_r = Ring(b, "lg", [128, H, GL], F32, NB2)
    k_r = Ring(b, "k", [128, H, GL], BF16, NB2)
    v_r = Ring(b, "vt", [128, NCH // 2, D], BF16, 2)
    bb_r = Ring(b, "bb", [128, H, GL], F32, NB2)
    e_r = Ring(b, "e", [128, H, GL], F32, 2)
    dl_r = Ring(b, "dl", [128, H, NCH], F32, 2)
    k2_r = Ring(b, "k2", [128, H, GL], BF16, NB2)
    k2t_r = Ring(b, "k2t", [128, NCH // 2, H * 128], BF16, 2)
    pt_r = Ring(b, "ptk", [128, 1024], BF16, 1, psum=True)
    pu_r = Ring(b, "pu", [128, 1024], F32, 1, psum=True)
    for gi in range(NGL):
        t0 = gi * GL
        lg, t_lg = lg_r.next(); k, t_k = k_r.next(); vt, t_vt = v_r.next()
        b.dma("sp", lg[:], LG[:, :, t0:t0 + GL].rearrange("h p t -> p h t"), reads=tl, writes=[t_lg])
        b.dma("sp", k[:], KG[:, :, t0:t0 + GL].rearrange("h p t -> p h t"), reads=tk, writes=[t_k])
        b.dma("sp", vt[:], VG[t0:t0 + GL, :].rearrange("(r p) f -> p r f", p=128), reads=tv, writes=[t_vt])
        if mode == 2:
            q, t_q = q_r.next(); sg, t_sg = sg_r.next()
            b.dma("sp", q[:], QG[:, :, t0:t0 + GL].rearrange("h p t -> p h t"), reads=tq, writes=[t_q])
            b.dma("sp", sg[:], SG[:, :, t0:t0 + GL].rearrange("c p t -> p c t"), reads=tsg, writes=[t_sg])
        bb, t_bb = bb_r.next()
        b.op("dve", lambda: nc.vector.tensor_tensor_scan(out=bb[:].rearrange("p h t -> p (h t)"), data0=rmask[:].rearrange("p h t -> p (h t)"),
                                                         data1=lg[:].rearrange("p h t -> p (h t)"), initial=0.0, op0=ALU.mult, op1=ALU.add),
             reads=[t_rm, t_lg], writes=[t_bb])
        bl = bb[:].rearrange("p h (c s) -> p h c s", s=64)[:, :, :, 63]
        dl, t_dl = dl_r.next()
        b.op("act", lambda: nc.scalar.activation(out=dl[:], in_=bl, func=AF.Exp), reads=[t_bb], writes=[t_dl])
        if mode == 1:
            b.op("dve", lambda: nc.vector.tensor_reduce(out=bred[:], in_=bl, axis=AX.X, op=ALU.add), reads=[t_bb], writes=[t_bs])
            b.op("dve", lambda: nc.vector.tensor_tensor(out=bsum[:, 0:H], in0=bsum[:, 0:H], in1=bred[:], op=ALU.add), reads=[t_bs], writes=[t_bs])
        e, t_e = e_r.next()
        for h in range(H):
            for c in range(NCH):
                b.op("act", lambda: nc.scalar.activation(out=e[:, h, c * 64:(c + 1) * 64], in_=bb[:, h, c * 64:(c + 1) * 64], func=AF.Exp, scale=-1.0,
                                                         bias=bb[:, h, c * 64 + 63:c * 64 + 64]), reads=[t_bb], writes=[t_e])
        k2, t_k2 = k2_r.next()
        b.op("dve", lambda: nc.vector.tensor_tensor(out=k2[:], in0=k[:], in1=e[:], op=ALU.mult), reads=[t_k, t_e], writes=[t_k2])
        if mode == 2:
            e2, t_e2 = e_r.next()
            b.op("act", lambda: nc.scalar.activation(out=e2[:], in_=bb[:], func=AF.Exp, scale=-1.0), reads=[t_bb], writes=[t_e2])
            kd, t_kd = kd_r.next()
            b.op("dve", lambda: nc.vector.tensor_tensor(out=kd[:], in0=k[:], in1=e2[:], op=ALU.mult), reads=[t_k, t_e2], writes=[t_kd])
            e3, t_e3 = e_r.next()
            b.op("act", lambda: nc.scalar.activation(out=e3[:], in_=bb[:], func=AF.Exp), reads=[t_bb], writes=[t_e3])
            qd, t_qd = qd_r.next()
            b.op("dve", lambda: nc.vector.tensor_tensor(out=qd[:], in0=q[:], in1=e3[:], op=ALU.mult), reads=[t_q, t_e3], writes=[t_qd])
        k2t, t_k2t = k2t_r.next()
        for r in range(NCH // 2):
            pt, t_pt = pt_r.next()
            for h in range(H):
                b.op("pe", lambda: nc.tensor.transpose(pt[:, h * 128:(h + 1) * 128], k2[:, h, r * 128:(r + 1) * 128], self.ident[:]),
                     reads=[t_k2, self.t_ident], writes=[t_pt])
            b.op("act", lambda: nc.scalar.copy(out=k2t[:, r, :], in_=pt[:, 0:H * 128]), reads=[t_pt], writes=[t_k2t])
        if mode == 2:
            att, t_att = att_r.next()
            for r in range(NCH // 2):
                pa, t_pa = pa_r.next()
                for cc in range(2):
                    c = r * 2 + cc
                    po_ = cc * 64
                    for h in range(H):
                        b.op("pe", lambda: nc.tensor.matmul(pa[po_:po_ + 64, h * 64:(h + 1) * 64], lhsT=kd[:, h, c * 64:(c + 1) * 64],
                                                            rhs=qd[:, h, c * 64:(c + 1) * 64], start=(h == 0), stop=True, tile_position=(0, po_)),
                             reads=[t_kd, t_qd], writes=[t_pa])
                b.op("dve", lambda: nc.vector.tensor_tensor(out=att[:, r, :], in0=pa[:, 0:H * 64], in1=tri[:, 0:H * 64], op=ALU.mult),
                     reads=[t_pa, t_tri], writes=[t_att])
            oraw, t_oraw = oraw_r.next()
        for c in range(NCH):
            r, po_ = c // 2, (c % 2) * 64
            if mode == 2:
                po, t_po = po_r.next()
                first = True
                for h in range(H):
                    for vv in range(VT):
                        mt = h * VT + vv
                        b.op("pe", lambda: nc.tensor.matmul(po[:, mt * 64:(mt + 1) * 64], lhsT=vt[po_:po_ + 64, r, h * V + vv * 128:h * V + (vv + 1) * 128],
                                                            rhs=att[po_:po_ + 64, r, h * 64:(h + 1) * 64], start=first, stop=False),
                             reads=[t_vt, t_att], writes=[t_po])
                        first = False
                        b.op("pe", lambda: nc.tensor.matmul(po[:, mt * 64:(mt + 1) * 64], lhsT=sbf[:, h, vv * 128:(vv + 1) * 128],
                                                            rhs=qd[:, h, c * 64:(c + 1) * 64], start=False, stop=True),
                             reads=[t_sbf, t_qd], writes=[t_po])
                b.op("act", lambda: nc.scalar.copy(out=oraw[:, :, c * 64:(c + 1) * 64], in_=po[:].rearrange("p (m t) -> p m t", m=8)),
                     reads=[t_po], writes=[t_oraw])
            pu, t_pu = pu_r.next()
            for h in range(H):
                b.op("pe", lambda: nc.tensor.matmul(pu[:, h * V:(h + 1) * V], lhsT=k2t[po_:po_ + 64, r, h * 128:(h + 1) * 128],
                                                    rhs=vt[po_:po_ + 64, r, h * V:(h + 1) * V], start=(h * V % 512 == 0), stop=True),
                     reads=[t_k2t, t_vt], writes=[t_pu])
            for h in range(H):
                b.op("dve", lambda: nc.vector.scalar_tensor_tensor(out=S[:, h, :], in0=S[:, h, :], scalar=dl[:, h, c:c + 1], in1=pu[:, h * V:(h + 1) * V],
                                                                   op0=ALU.mult, op1=ALU.add), reads=[t_S, t_dl, t_pu], writes=[t_S])
            if mode == 2:
                sbf, t_sbf = sbf_r.next()
                b.op("act", lambda: nc.scalar.copy(out=sbf[:], in_=S[:]), reads=[t_S], writes=[t_sbf])
        if mode == 2:
            sq, t_sq = sq_r.next()
            b.op("act", lambda: nc.scalar.activation(out=sq[:], in_=oraw[:], func=AF.Square), reads=[t_oraw], writes=[t_sq])
            otg, t_otg = ot_r.next()
            for h in range(H):
                pss, t_pss = po_r.next()
                for vv in range(VT):
                    b.op("pe", lambda: nc.tensor.matmul(pss[:, 0:GL], lhsT=self.ones[:], rhs=sq[:, h * VT + vv, :], start=(vv == 0), stop=(vv == VT - 1)),
                         reads=[self.t_ones, t_sq], writes=[t_pss])
                rs, t_rs = rs_r.next()
                b.op("act", lambda: nc.scalar.activation(out=rs[:], in_=pss[:, 0:GL], func=AF.Sqrt, scale=1.0 / V, bias=1e-6), reads=[t_pss], writes=[t_rs])
                b.op("dve", lambda: nc.vector.reciprocal(out=rs[:], in_=rs[:]), reads=[t_rs], writes=[t_rs])
                for vv in range(VT):
                    mt = h * VT + vv
                    t1, t_t1 = t1_r.next()
                    b.op("dve", lambda: nc.vector.tensor_tensor(out=t1[:], in0=oraw[:, mt, :], in1=rs[:], op=ALU.mult), reads=[t_oraw, t_rs], writes=[t_t1])
                    b.op("dve", lambda: nc.vector.scalar_tensor_tensor(out=otg[:, mt, :], in0=t1[:], scalar=wn[:, vv:vv + 1], in1=sg[:, mt, :],
                                                                       op0=ALU.mult, op1=ALU.mult), reads=[t_t1, t_wn, t_sg], writes=[t_otg])
            b.dma("pool", OT[:, :, t0:t0 + GL].rearrange("c p t -> p c t"), otg[:], reads=[t_otg], writes=[self.dt_("OT" + sfx, ("g", gi))])
    if mode == 1:
        SL = self.dr("SLOC" + sfx, [128, H * V], F32)
        DT = self.dr("DTOT" + sfx, [128, 128], F32)
        b.op("act", lambda: nc.scalar.activation(out=bsum[:, 0:H], in_=bsum[:, 0:H], func=AF.Exp), reads=[t_bs], writes=[t_bs])
        b.dma("pool", SL[:, :], S[:].rearrange("p h v -> p (h v)"), reads=[t_S], writes=[self.dt_("SLOC" + sfx)], is_output=True)
        b.dma("pool", DT[:, :], bsum[:], reads=[t_bs], writes=[self.dt_("DTOT" + sfx)], is_output=True)
    b.pop()


Prog.lin_pass = _lin_pass


def bf(a): return np.asarray(a, np.float32).astype(NPBF)
def split_bf(x, n):
    parts = []; r = np.asarray(x, np.float64)
    for _ in range(n):
        p = r.astype(np.float32).astype(NPBF); parts.append(p); r = r - p.astype(np.float64)
    return parts
def slopes():
    s = np.exp2(-8.0 * np.arange(1, 17, dtype=np.float64) / 16)
    s1, s2 = split_bf(s, 2)
    return s1, s2, s1.astype(np.float64) + s2.astype(np.float64)
def qaug(tq_rel):
    s1, s2, sp = slopes(); T = len(tq_rel)
    out = np.zeros((4, 7, 4, T), NPBF)
    for h in range(16):
        kvh, g = h // 4, h % 4
        A = split_bf(-sp[h] * tq_rel.astype(np.float64), 3)
        out[kvh, 0, g], out[kvh, 1, g], out[kvh, 2, g] = A
        out[kvh, 3, g] = s1[h]; out[kvh, 4, g] = s2[h]; out[kvh, 5, g] = s1[h]; out[kvh, 6, g] = s2[h]
    return out
def kaug(tk_rel):
    T = len(tk_rel); out = np.zeros((7, T), NPBF)
    hi = 256 * np.floor(tk_rel / 256.0); lo = tk_rel - hi
    out[0:3] = 1.0; out[3] = bf(hi); out[4] = bf(hi); out[5] = bf(lo); out[6] = bf(lo)
    assert np.all(out[3].astype(np.float64) == hi) and np.all(out[5].astype(np.float64) == lo)
    return out
def swamask(first):
    k = np.arange(128)[:, None]; q = np.arange(128)[None, :]
    prev = np.where(k > q, 0.0, NEG); own = np.where(k <= q, 0.0, NEG)
    m0 = np.full((128, 128), NEG) if first else prev
    return bf(np.stack([np.tile(m, (1, 4)) for m in (m0, prev, own)]))
def onehot():
    t = np.arange(64 * 256); rho = t // 256
    out = np.zeros((32, 64 * 256), NPBF); out[rho % 32, t] = 1.0
    return out
def valid(cseq):
    t = np.arange(4096)[:, None]; rho = np.arange(64)[None, :]
    gb = 16 * cseq - 48 + rho
    ok = (gb >= 0) & (rho < 48 + t // 256)
    return np.where(ok, 0.0, NEG).astype(np.float32)
def window(full, cseq, axis, blk):
    pad = [(0, 0)] * full.ndim; pad[axis] = (48 * blk, 0)
    p = np.pad(full, pad)
    sl = [slice(None)] * full.ndim; sl[axis] = slice(cseq * 16 * blk, (cseq * 16 + 64) * blk)
    return np.ascontiguousarray(p[tuple(sl)])
def tri():
    s = np.arange(128)[:, None] % 64; t = np.arange(512)[None, :] % 64
    return bf((s <= t).astype(np.float32))


def _common(inputs):
    return {"norm_mix": inputs["norm_mix"], "norm_ffn": inputs["norm_ffn"], "final_norm": inputs["final_norm"],
            "c_ident": np.eye(128).astype(NPBF), "c_tri": tri()}


BASE_IN = ["xa", "norm_mix", "norm_ffn", "final_norm", "c_ident"]


def _run(p, ims):
    p.b.finish()
    names = [k for k in p.dram if k in p.ext_in]
    ims = [{k: np.ascontiguousarray(im[k]) for k in names} for im in ims]
    return run_bass_kernel_spmd(p.nc, ims, core_ids=list(range(NCORE))).results


def _states(r, L, c, inputs_extra):
    cs = c % 4
    out = {}
    for i in (1, 2, 3):
        out[f"SP{i}"] = r[c - i][f"SLOC{L}"] if cs - i >= 0 else np.zeros((128, 1024), np.float32)
    for i in (1, 2):
        out[f"DP{i}"] = r[c - i][f"DTOT{L}"] if cs - i >= 0 else np.zeros((128, 128), np.float32)
    out.update(inputs_extra)
    return out


def build_l1():
    p = Prog(ext_in=BASE_IN + ["xhalo", "swa_w_in", "swa_w_out", "swa_sinks", "ffn_w_gate_up", "ffn_w_down", "moba_w_in",
                               "c_qaug", "c_kaug0", "c_swamask"],
             ext_out=["x1", "QT1", "KT1", "V1", "KM"])
    p.load_consts()
    p.cast_w("swa_w_in", [1, D, 1536], "win0_bf", sel=0)
    p.cast_w("swa_w_out", [1, D, D], "wout0_bf", sel=0)
    p.cast_w("ffn_w_gate_up", [1, D, 2 * DFF], "wgu0_bf", sel=0)
    p.cast_w("ffn_w_down", [1, DFF, D], "wd0_bf", sel=0)
    p.cast_w("moba_w_in", [1, D, 1536], "win1_bf", sel=0)
    p.attn_proj(0, "xa", "win0_bf", 71, 64, halo_name="xhalo")
    p.stage_swa()
    p.stage_post(0, "xa", "x1i", "OT0", "wout0_bf", x_out2="x1")
    p.attn_proj(1, "x1i", "win1_bf", 103, 96, moba=True)
    return p


def build_l2():
    p = Prog(ext_in=BASE_IN + ["QT1", "GKT", "GV", "GKM", "moba_w_out", "ffn_w_gate_up", "ffn_w_down", "gla_w_in", "gla_w_decay_up", "gla_b_decay",
                               "c_qaug", "c_kaug1", "c_onehot", "c_valid", "c_swamask", "c_tri"],
             ext_out=["x2", "SLOC2", "DTOT2"])
    p.load_consts()
    p.cast_w("moba_w_out", [1, D, D], "wout1_bf", sel=0)
    p.cast_w("ffn_w_gate_up", [1, D, 2 * DFF], "wgu1_bf", sel=0)
    p.cast_w("ffn_w_down", [1, DFF, D], "wd1_bf", sel=0)
    p.cast_w("gla_w_in", [1, D, 3088], "win2_bf", sel=0)
    p.stage_moba()
    p.stage_post(1, "xa", "x2i", "OT1", "wout1_bf", x_out2="x2")
    p.lin_proj(2, 2, "x2i")
    p.lin_pass(2, 2, 1)
    return p


def build_l3():
    p = Prog(ext_in=BASE_IN + ["SP1", "SP2", "SP3", "DP1", "DP2", "gla_w_in", "gla_w_decay_up", "gla_b_decay", "gla_out_norm", "gla_w_out",
                               "ffn_w_gate_up", "ffn_w_down", "hgrn_w_in", "hgrn_lb_logits", "c_tri"],
             ext_out=["x3", "SLOC3", "DTOT3"])
    p.load_consts()
    p.cast_w("gla_w_in", [1, D, 3088], "win2_bf", sel=0)
    p.cast_w("gla_w_out", [1, D, D], "wout2_bf", sel=0)
    p.cast_w("ffn_w_gate_up", [1, D, 2 * DFF], "wgu2_bf", sel=0)
    p.cast_w("ffn_w_down", [1, DFF, D], "wd2_bf", sel=0)
    p.cast_w("hgrn_w_in", [1, D, 4096], "win3_bf", sel=0)
    p.lin_proj(2, 2, "xa")
    p.lin_pass(2, 2, 2)
    p.stage_post(2, "xa", "x3i", "OT2", "wout2_bf", x_out2="x3")
    p.lin_proj(3, 3, "x3i")
    p.lin_pass(3, 3, 1)
    return p


def build_l4():
    p = Prog(ext_in=BASE_IN + ["SP1", "SP2", "SP3", "DP1", "DP2", "hgrn_w_in", "hgrn_lb_logits", "hgrn_out_norm", "hgrn_w_out",
                               "ffn_w_gate_up", "ffn_w_down", "c_tri"],
             ext_out=["out"])
    p.load_consts()
    p.cast_w("hgrn_w_in", [1, D, 4096], "win3_bf", sel=0)
    p.cast_w("hgrn_w_out", [1, D, D], "wout3_bf", sel=0)
    p.cast_w("ffn_w_gate_up", [1, D, 2 * DFF], "wgu3_bf", sel=0)
    p.cast_w("ffn_w_down", [1, DFF, D], "wd3_bf", sel=0)
    p.lin_proj(3, 3, "xa")
    p.lin_pass(3, 3, 2)
    p.stage_post(3, "xa", "x4i", "OT3", "wout3_bf", final=True, out_name="out")
    return p


def kernel(**inputs):
    inputs = {k: np.ascontiguousarray(np.asarray(v)) for k, v in inputs.items()}
    X = inputs["x"].reshape(2 * SEQ, D)
    com = _common(inputs)
    qa = qaug(np.arange(TC))
    p = build_l1()
    ims = []
    for c in range(NCORE):
        first = (c % 4 == 0)
        xh = np.zeros((128, D), np.float32) if first else X[c * TC - 128:c * TC]
        ims.append(dict(com, xa=X[c * TC:(c + 1) * TC], xhalo=xh, swa_w_in=inputs["swa_w_in"], swa_w_out=inputs["swa_w_out"],
                        swa_sinks=inputs["swa_sinks"], ffn_w_gate_up=inputs["ffn_w_gate_up"][0:1], ffn_w_down=inputs["ffn_w_down"][0:1],
                        moba_w_in=inputs["moba_w_in"], c_qaug=qa, c_kaug0=kaug(np.arange(-128, TC)), c_swamask=swamask(first)))
    r1 = _run(p, ims)
    GKT, GV, GKM = [], [], []
    for c in range(NCORE):
        s, cs = c // 4, c % 4
        Kseq = np.concatenate([r1[s * 4 + i]["KT1"] for i in range(4)], axis=2)
        Vseq = np.concatenate([r1[s * 4 + i]["V1"] for i in range(4)], axis=0)
        KMseq = np.concatenate([r1[s * 4 + i]["KM"][:, :, :16] for i in range(4)], axis=2)
        GKT.append(window(Kseq, cs, 2, 256)); GV.append(window(Vseq, cs, 0, 256)); GKM.append(window(KMseq, cs, 2, 1))
    p = build_l2()
    ka1 = kaug(np.arange(-48 * 256, 16 * 256)); oh = onehot(); sm = swamask(False)
    ims = [dict(com, xa=r1[c]["x1"], QT1=r1[c]["QT1"], GKT=GKT[c], GV=GV[c], GKM=GKM[c], moba_w_out=inputs["moba_w_out"],
                ffn_w_gate_up=inputs["ffn_w_gate_up"][1:2], ffn_w_down=inputs["ffn_w_down"][1:2], gla_w_in=inputs["gla_w_in"],
                gla_w_decay_up=inputs["gla_w_decay_up"], gla_b_decay=inputs["gla_b_decay"],
                c_qaug=qa, c_kaug1=ka1, c_onehot=oh, c_valid=valid(c % 4), c_swamask=sm) for c in range(NCORE)]
    r2 = _run(p, ims)
    del r1, GKT, GV, GKM
    p = build_l3()
    ims = [dict(com, xa=r2[c]["x2"], **_states(r2, 2, c, dict(
        gla_w_in=inputs["gla_w_in"], gla_w_decay_up=inputs["gla_w_decay_up"], gla_b_decay=inputs["gla_b_decay"], gla_out_norm=inputs["gla_out_norm"],
        gla_w_out=inputs["gla_w_out"], ffn_w_gate_up=inputs["ffn_w_gate_up"][2:3], ffn_w_down=inputs["ffn_w_down"][2:3],
        hgrn_w_in=inputs["hgrn_w_in"], hgrn_lb_logits=inputs["hgrn_lb_logits"]))) for c in range(NCORE)]
    r3 = _run(p, ims)
    del r2
    p = build_l4()
    ims = [dict(com, xa=r3[c]["x3"], **_states(r3, 3, c, dict(
        hgrn_w_in=inputs["hgrn_w_in"], hgrn_lb_logits=inputs["hgrn_lb_logits"], hgrn_out_norm=inputs["hgrn_out_norm"], hgrn_w_out=inputs["hgrn_w_out"],
        ffn_w_gate_up=inputs["ffn_w_gate_up"][3:4], ffn_w_down=inputs["ffn_w_down"][3:4]))) for c in range(NCORE)]
    r4 = _run(p, ims)
    out = np.concatenate([r4[c]["out"] for c in range(NCORE)], axis=0).reshape(2, SEQ, D).astype(np.float32)
    return out
```
